# Optimizing a Trainium2 kernel written in Bass

```python
import jax, jax.numpy as jnp
from jax import lax
import numpy as np

D_MODEL = 1024
BATCH = 4
SEQ = 8192
DEPTH = 2

HEAD_DIM = 64
Q_BLOCK = 128
RMS_EPS = 1e-6
ROPE_THETA = 10000.0
FOX_HEADS = 8
MLA_HEADS = 8
MLA_Q_RANK = 384
MLA_KV_RANK = 256
MLA_NOPE_DIM = 64
MLA_ROPE_DIM = 32
MLA_V_DIM = 64
SWA_HEADS = 8
SWA_KV_HEADS = 2
WINDOW = 128
CONV_CHANNELS = 512
CONV_WIDTH = 3
D_FF = 3584
N_EXPERTS = 8
TOP_K = 2

SPLIT_EVEN = (FOX_HEADS * HEAD_DIM, FOX_HEADS * HEAD_DIM, FOX_HEADS * HEAD_DIM, FOX_HEADS,
              MLA_Q_RANK, MLA_KV_RANK, MLA_ROPE_DIM)
SPLIT_ODD = (SWA_HEADS * HEAD_DIM, SWA_KV_HEADS * HEAD_DIM, SWA_KV_HEADS * HEAD_DIM,
             CONV_CHANNELS, CONV_CHANNELS, CONV_CHANNELS)
IN_EVEN = 3 * FOX_HEADS * HEAD_DIM + FOX_HEADS + MLA_Q_RANK + MLA_KV_RANK + MLA_ROPE_DIM
IN_ODD = SWA_HEADS * HEAD_DIM + 2 * SWA_KV_HEADS * HEAD_DIM + 3 * CONV_CHANNELS
MIX_EVEN = FOX_HEADS * HEAD_DIM + MLA_HEADS * MLA_V_DIM
MIX_ODD = SWA_HEADS * HEAD_DIM + CONV_CHANNELS

kernel_name = 'hybrid_fox_mla_swa_conv_moe'


def rms_norm(x, g):
    xf = x.astype(jnp.float32)
    y = xf * lax.rsqrt(jnp.mean(xf * xf, axis=-1, keepdims=True) + RMS_EPS)
    return (y * g.astype(jnp.float32)).astype(x.dtype)


def split_cols(x, sizes):
    out, start = [], 0
    for size in sizes:
        out.append(x[..., start:start + size])
        start += size
    return out


def rope_tables(positions, dim):
    inv_freq = ROPE_THETA ** (-jnp.arange(0, dim, 2, dtype=jnp.float32) / dim)
    ang = positions.astype(jnp.float32)[..., None] * inv_freq
    return jnp.cos(ang), jnp.sin(ang)


def apply_rope(x, cos, sin):
    x1, x2 = jnp.split(x, 2, axis=-1)
    c = cos[:, :, None, :].astype(x.dtype)
    s = sin[:, :, None, :].astype(x.dtype)
    return jnp.concatenate([x1 * c - x2 * s, x2 * c + x1 * s], axis=-1)


def causal_block_attention(q, k, v, scale, decay_cum=None):
    B, S, H, Dk = q.shape
    nb = S // Q_BLOCK
    key_pos = jnp.arange(S)
    q_blocks = jnp.moveaxis(q.reshape(B, nb, Q_BLOCK, H, Dk), 1, 0)
    if decay_cum is not None:
        c_keys = jnp.swapaxes(decay_cum, 1, 2)
        c_blocks = jnp.moveaxis(decay_cum.reshape(B, nb, Q_BLOCK, H), 1, 0)
        xs = (jnp.arange(nb), q_blocks, c_blocks)
    else:
        xs = (jnp.arange(nb), q_blocks)

    def attend(blk):
        n, q_blk = blk[0], blk[1]
        s = jnp.einsum('bqhd,bkhd->bhqk', q_blk, k, preferred_element_type=jnp.float32) * scale
        if decay_cum is not None:
            s = s + jnp.swapaxes(blk[2], 1, 2)[..., :, None] - c_keys[:, :, None, :]
        q_pos = n * Q_BLOCK + jnp.arange(Q_BLOCK)
        s = jnp.where(key_pos[None, :] <= q_pos[:, None], s, -jnp.inf)
        p = jax.nn.softmax(s, axis=-1)
        return jnp.einsum('bhqk,bkhd->bqhd', p.astype(v.dtype), v)

    out = lax.map(attend, xs)
    return jnp.moveaxis(out, 0, 1).reshape(B, S, H, v.shape[-1])


def sliding_window_sink_attention(q, k, v, sinks, scale):
    B, S, Hq, D = q.shape
    Hkv = k.shape[2]
    G = Hq // Hkv
    nb = S // Q_BLOCK
    qb = q.reshape(B, nb, Q_BLOCK, Hkv, G, D)
    kb = k.reshape(B, nb, Q_BLOCK, Hkv, D)
    vb = v.reshape(B, nb, Q_BLOCK, Hkv, D)
    pad = ((0, 0), (1, 0), (0, 0), (0, 0), (0, 0))
    k_band = jnp.concatenate([jnp.pad(kb, pad)[:, :-1], kb], axis=2)
    v_band = jnp.concatenate([jnp.pad(vb, pad)[:, :-1], vb], axis=2)
    s = jnp.einsum('bnqhgd,bnkhd->bnhgqk', qb, k_band, preferred_element_type=jnp.float32) * scale
    kidx = jnp.arange(2 * Q_BLOCK)
    rel = (Q_BLOCK + jnp.arange(Q_BLOCK))[:, None] - kidx[None, :]
    in_window = (rel >= 0) & (rel < WINDOW)
    real_key = (jnp.arange(nb)[:, None] > 0) | (kidx[None, :] >= Q_BLOCK)
    mask = in_window[None, :, :] & real_key[:, None, :]
    s = jnp.where(mask[None, :, None, None], s, -jnp.inf)
    sink = sinks.astype(jnp.float32).reshape(Hkv, G)[None, None, :, :, None, None]
    m = jnp.maximum(jnp.max(s, axis=-1, keepdims=True), sink)
    e = jnp.exp(s - m)
    p = e / (jnp.sum(e, axis=-1, keepdims=True) + jnp.exp(sink - m))
    out = jnp.einsum('bnhgqk,bnkhd->bnqhgd', p.astype(v.dtype), v_band)
    return out.reshape(B, S, Hq * D)


def causal_depthwise_conv(u, w):
    C = u.shape[-1]
    return lax.conv_general_dilated(
        u, w[:, None, :].astype(u.dtype), window_strides=(1,),
        padding=[(CONV_WIDTH - 1, 0)], dimension_numbers=('NWC', 'WIO', 'NWC'),
        feature_group_count=C)


def swiglu(x, w_gate, w_up, w_down):
    return (jax.nn.silu(x @ w_gate) * (x @ w_up)) @ w_down


def moe_swiglu(x, w_router, w_gate, w_up, w_down):
    B, S, D = x.shape
    xt = x.reshape(B * S, D)
    logits = (xt @ w_router).astype(jnp.float32)
    top_vals, top_idx = lax.top_k(logits, TOP_K)
    top_w = jax.nn.softmax(top_vals, axis=-1)
    gates = jnp.einsum('nk,nke->ne', top_w, jax.nn.one_hot(top_idx, N_EXPERTS, dtype=jnp.float32))
    gates = gates.astype(x.dtype)
    y = jnp.zeros_like(xt)
    for e in range(N_EXPERTS):
        y = y + gates[:, e:e + 1] * swiglu(xt, w_gate[e], w_up[e], w_down[e])
    return y.reshape(B, S, D)


def even_layer(h, cos_m, sin_m, norm_mix, w_in, b_forget, q_norm, w_q_up, kv_norm, w_kv_up,
               w_out, norm_ffn, w_gate, w_up, w_down):
    B, S, _ = h.shape
    xn = rms_norm(h, norm_mix)
    fq, fk, fv, f_logit, c_q, c_kv, k_rope = split_cols(xn @ w_in, SPLIT_EVEN)
    log_f = jax.nn.log_sigmoid(f_logit.astype(jnp.float32) + b_forget.astype(jnp.float32))
    decay_cum = jnp.cumsum(log_f, axis=1)
    o_fox = causal_block_attention(
        fq.reshape(B, S, FOX_HEADS, HEAD_DIM), fk.reshape(B, S, FOX_HEADS, HEAD_DIM),
        fv.reshape(B, S, FOX_HEADS, HEAD_DIM), HEAD_DIM ** -0.5, decay_cum)
    q = (rms_norm(c_q, q_norm) @ w_q_up).reshape(B, S, MLA_HEADS, MLA_NOPE_DIM + MLA_ROPE_DIM)
    q_pe = apply_rope(q[..., MLA_NOPE_DIM:], cos_m, sin_m)
    kv = (rms_norm(c_kv, kv_norm) @ w_kv_up).reshape(B, S, MLA_HEADS, MLA_NOPE_DIM + MLA_V_DIM)
    k_nope, v = kv[..., :MLA_NOPE_DIM], kv[..., MLA_NOPE_DIM:]
    k_pe = apply_rope(k_rope.reshape(B, S, 1, MLA_ROPE_DIM), cos_m, sin_m)
    q_full = jnp.concatenate([q[..., :MLA_NOPE_DIM], q_pe], axis=-1)
    k_full = jnp.concatenate(
        [k_nope, jnp.broadcast_to(k_pe, (B, S, MLA_HEADS, MLA_ROPE_DIM))], axis=-1)
    o_mla = causal_block_attention(q_full, k_full, v, (MLA_NOPE_DIM + MLA_ROPE_DIM) ** -0.5)
    mix = jnp.concatenate([o_fox.reshape(B, S, -1), o_mla.reshape(B, S, -1)], axis=-1)
    h = h + mix @ w_out
    return h + swiglu(rms_norm(h, norm_ffn), w_gate, w_up, w_down)


def odd_layer(h, cos_s, sin_s, norm_mix, w_in, sinks, conv_w, w_out, norm_ffn, w_router,
              w_gate, w_up, w_down):
    B, S, _ = h.shape
    xn = rms_norm(h, norm_mix)
    sq, sk, sv, gate_b, gate_c, xc = split_cols(xn @ w_in, SPLIT_ODD)
    q = apply_rope(sq.reshape(B, S, SWA_HEADS, HEAD_DIM), cos_s, sin_s)
    k = apply_rope(sk.reshape(B, S, SWA_KV_HEADS, HEAD_DIM), cos_s, sin_s)
    v = sv.reshape(B, S, SWA_KV_HEADS, HEAD_DIM)
    o_swa = sliding_window_sink_attention(q, k, v, sinks, HEAD_DIM ** -0.5)
    o_conv = gate_b * causal_depthwise_conv(gate_c * xc, conv_w)
    mix = jnp.concatenate([o_swa, o_conv], axis=-1)
    h = h + mix @ w_out
    return h + moe_swiglu(rms_norm(h, norm_ffn), w_router, w_gate, w_up, w_down)


def setup_inputs(seed: int = 0) -> dict:
    key = jax.random.key(seed)
    ks = jax.random.split(key, 26)

    def w(k, shape, fan_in):
        return jax.random.normal(k, shape, jnp.float32) * fan_in ** -0.5

    def gain(k, n):
        return 1.0 + 0.02 * jax.random.normal(k, (n,), jnp.float32)

    x = jax.random.normal(ks[0], (BATCH, SEQ, D_MODEL), jnp.float32)
    offsets = jax.random.randint(ks[1], (BATCH, 1), 0, 4096, dtype=jnp.int32)
    positions = (jnp.arange(SEQ, dtype=jnp.int32)[None, :] + offsets).astype(jnp.int32)
    return {
        'x': x,
        'positions': positions,
        'l0_norm_mix': gain(ks[2], D_MODEL),
        'l0_w_in': w(ks[3], (D_MODEL, IN_EVEN), D_MODEL),
        'l0_b_forget': 2.0 + 0.1 * jax.random.normal(ks[4], (FOX_HEADS,), jnp.float32),
        'l0_q_norm': gain(ks[5], MLA_Q_RANK),
        'l0_w_q_up': w(ks[6], (MLA_Q_RANK, MLA_HEADS * (MLA_NOPE_DIM + MLA_ROPE_DIM)), MLA_Q_RANK),
        'l0_kv_norm': gain(ks[7], MLA_KV_RANK),
        'l0_w_kv_up': w(ks[8], (MLA_KV_RANK, MLA_HEADS * (MLA_NOPE_DIM + MLA_V_DIM)), MLA_KV_RANK),
        'l0_w_out': w(ks[9], (MIX_EVEN, D_MODEL), MIX_EVEN),
        'l0_norm_ffn': gain(ks[10], D_MODEL),
        'l0_w_gate': w(ks[11], (D_MODEL, D_FF), D_MODEL),
        'l0_w_up': w(ks[12], (D_MODEL, D_FF), D_MODEL),
        'l0_w_down': w(ks[13], (D_FF, D_MODEL), D_FF),
        'l1_norm_mix': gain(ks[14], D_MODEL),
        'l1_w_in': w(ks[15], (D_MODEL, IN_ODD), D_MODEL),
        'l1_sinks': 0.5 * jax.random.normal(ks[16], (SWA_HEADS,), jnp.float32),
        'l1_conv_w': w(ks[17], (CONV_WIDTH, CONV_CHANNELS), CONV_WIDTH),
        'l1_w_out': w(ks[18], (MIX_ODD, D_MODEL), MIX_ODD),
        'l1_norm_ffn': gain(ks[19], D_MODEL),
        'l1_w_router': w(ks[20], (D_MODEL, N_EXPERTS), D_MODEL),
        'l1_w_gate': w(ks[21], (N_EXPERTS, D_MODEL, D_FF), D_MODEL),
        'l1_w_up': w(ks[22], (N_EXPERTS, D_MODEL, D_FF), D_MODEL),
        'l1_w_down': w(ks[23], (N_EXPERTS, D_FF, D_MODEL), D_FF),
        'final_norm': gain(ks[24], D_MODEL),
    }


def reference(x, positions, l0_norm_mix, l0_w_in, l0_b_forget, l0_q_norm, l0_w_q_up, l0_kv_norm,
              l0_w_kv_up, l0_w_out, l0_norm_ffn, l0_w_gate, l0_w_up, l0_w_down,
              l1_norm_mix, l1_w_in, l1_sinks, l1_conv_w, l1_w_out, l1_norm_ffn, l1_w_router,
              l1_w_gate, l1_w_up, l1_w_down, final_norm):
    cos_m, sin_m = rope_tables(positions, MLA_ROPE_DIM)
    cos_s, sin_s = rope_tables(positions, HEAD_DIM)
    layer_params = (
        (l0_norm_mix, l0_w_in, l0_b_forget, l0_q_norm, l0_w_q_up, l0_kv_norm, l0_w_kv_up,
         l0_w_out, l0_norm_ffn, l0_w_gate, l0_w_up, l0_w_down),
        (l1_norm_mix, l1_w_in, l1_sinks, l1_conv_w, l1_w_out, l1_norm_ffn, l1_w_router,
         l1_w_gate, l1_w_up, l1_w_down),
    )
    h = x
    for i in range(DEPTH):
        if i % 2 == 0:
            h = even_layer(h, cos_m, sin_m, *layer_params[i])
        else:
            h = odd_layer(h, cos_s, sin_s, *layer_params[i])
    return rms_norm(h, final_norm)
```

```python
import math, os
import numpy as np
DBG = os.environ.get('KDBG', '')
from contextlib import ExitStack
import concourse.bass as bass
import concourse.mybir as mybir
from concourse.bass_utils import run_bass_kernel_spmd

F32 = mybir.dt.float32; BF16 = mybir.dt.bfloat16; I32 = mybir.dt.int32
AF = mybir.ActivationFunctionType; ALU = mybir.AluOpType
ENGS = ('sync', 'scalar', 'vector', 'gpsimd', 'tensor')
EPS = 1e-6
NEG = -30000.0
MAGIC = 12582912.0
DFF = 3584; NF = 28; NE = 8


class Sem:
    def __init__(self, P, name):
        self.h = P.stack.enter_context(P.nc.semaphore(name)); self.n = 0


class Buf:
    def __init__(self, P, t):
        self.P = P; self.t = t; self.w = None; self.r = {}; self._sem = None

    def __getitem__(self, k):
        return self.t[k]

    @property
    def sem(self):
        if self._sem is None:
            self._sem = self.P.newsem()
        return self._sem


class _Rec:
    def __getattr__(self, name):
        def f(*a, **kw):
            return (name, a, kw)
        return f


REC = _Rec()


def _play(e, rec):
    return getattr(e, rec[0])(*rec[1], **rec[2])


class Prog:
    def __init__(self, nc, stack):
        self.nc = nc; self.stack = stack
        self.q = {k: [] for k in ENGS}
        self.waited = {k: {} for k in ENGS}
        self.sems = []
        self.esem = {k: self.newsem('e_' + k) for k in ('scalar', 'vector', 'gpsimd', 'tensor')}
        self.free_sems = []

    def newsem(self, name=None):
        s = Sem(self, name or f's{len(self.sems)}'); self.sems.append(s); return s

    def _waits(self, eng, deps):
        w = self.waited[eng]
        for d in deps:
            if d is None: continue
            s, v = d
            if eng == 'tensor' and s is self.esem['tensor']: continue
            if w.get(id(s), 0) >= v: continue
            w[id(s)] = v
            self.q[eng].append(lambda e, s=s, v=v: e.wait_ge(s.h, v))

    def _deps(self, R, W):
        deps = []
        for b in R: deps.append(b.w)
        for b in W:
            deps.append(b.w)
            deps += [(s, v) for (s, v) in b.r.values()]
        return deps

    def _commit(self, tok, R, W):
        for b in R:
            b.r[id(tok[0])] = tok
        for b in W:
            b.w = tok; b.r = {}

    def op(self, eng, fn, R=(), W=()):
        self._waits(eng, self._deps(R, W))
        s = self.esem[eng]; s.n += 1; tok = (s, s.n)
        rec = fn(REC)
        self.q[eng].append(lambda e, rec=rec, s=s: _play(e, rec).then_inc(s.h, 1))
        self._commit(tok, R, W)
        return tok

    def mm(self, items, R=(), W=()):
        self._waits('tensor', self._deps(R, W))
        s = self.esem['tensor']; s.n += 1; tok = (s, s.n)
        for i, fn in enumerate(items):
            rec = fn(REC)
            if i == len(items) - 1:
                self.q['tensor'].append(lambda e, rec=rec, s=s: _play(e, rec).then_inc(s.h, 1))
            else:
                self.q['tensor'].append(lambda e, rec=rec: _play(e, rec))
        self._commit(tok, R, W)
        return tok

    def dma(self, eng, out, in_, R=(), W=(), sem=None, **kw):
        self._waits(eng, self._deps(R, W))
        if sem is None:
            sem = (W[0] if W else R[0]).sem
        sem.n += 16; tok = (sem, sem.n)
        self.q[eng].append(lambda e, sem=sem, out=out, in_=in_, kw=kw:
                           e.dma_start(out=out, in_=in_, **kw).then_inc(sem.h, 16))
        self._commit(tok, R, W)
        return tok

    def barrier(self):
        for eng in ENGS:
            self._waits(eng, [(s, s.n) for s in self.sems if s.n > 0])

    def emit(self):
        with self.nc.Block() as block:
            for k in ENGS:
                getattr(block, k)(lambda e, k=k: [f(e) for f in self.q[k]])
        self.q = {k: [] for k in ENGS}


def build(NBO, upto=99):
    NBP = NBO; NKB = 2 * NBO; NQB = NBO + 1; NK = NKB * 128; NQ = NQB * 128
    QB0 = NBP - 1
    nc = bass.Bass("TRN2", target_bir_lowering=False)

    def din(name, shape, dt=F32):
        return nc.dram_tensor(name, list(shape), dt, kind="ExternalInput").ap()

    def dscr(name, shape, dt):
        kind = "ExternalOutput" if ('dump' in DBG and name in DBG) else "Internal"
        return nc.dram_tensor(name, list(shape), dt, kind=kind).ap()

    xk = din("xk", [NK, 1024]); valid = din("valid", [128, NKB]); pos = din("pos", [128, NKB], I32)
    g0mix = din("g0mix", [128, 8]); g0ffn = din("g0ffn", [128, 8]); g1mix = din("g1mix", [128, 8]); g1ffn = din("g1ffn", [128, 8])
    qng = din("qng", [128, 3]); kvng = din("kvng", [128, 2])
    w_in0 = din("w_in0", [1024, 2216]); bfor = din("bfor", [8]); wqup = din("wqup", [384, 768]); wkvup = din("wkvup", [256, 1024])
    wout0 = din("wout0", [1024, 1024]); wg0 = din("wg0", [1024, DFF]); wu0 = din("wu0", [1024, DFF]); wd0 = din("wd0", [DFF, 1024])
    w_in1 = din("w_in1", [1024, 2304]); sinks = din("sinks", [8]); convw = din("convw", [3, 512]); wout1 = din("wout1", [1024, 1024])
    wrT = din("wrT", [8, 1024]); g1ffn_row = din("g1ffn_row", [1024]); gfin_row = din("gfin_row", [1024])
    wg1 = din("wg1", [NE, 1024, DFF]); wu1 = din("wu1", [NE, 1024, DFF]); wd1 = din("wd1", [NE, DFF, 1024])
    c_ident = din("c_ident", [128, 128]); c_mown = din("c_mown", [128, 128]); c_mprev = din("c_mprev", [128, 128])
    c_U = din("c_U", [128, 128]); c_SU = din("c_SU", [NKB, NKB]); c_eye = din("c_eye", [NKB, NKB])
    c_invm = din("c_invm", [128, 16]); c_invs = din("c_invs", [128, 32])
    c_sh = din("c_sh", [4, 128, 128])
    out = nc.dram_tensor("out", [NBO * 128, 1024], F32, kind="ExternalOutput").ap()

    qf = dscr("qf", [8, 64, NQ], BF16); kf = dscr("kf", [8, 64, NK], BF16); vf = dscr("vf", [NK, 512], BF16)
    qm = dscr("qm", [8, 96, NQ], BF16); km = dscr("km", [8, 96, NK], BF16); vm = dscr("vm", [NK, 512], BF16)
    mixT = dscr("mixT", [1024, NQ], BF16); h1 = dscr("h1", [NQ, 1024], F32); xn2T = dscr("xn2T", [1024, NQ], BF16)
    h2 = dscr("h2", [NQ, 1024], F32); mix1T = dscr("mix1T", [1024, NBO * 128], BF16)
    h3 = dscr("h3", [NBO * 128, 1024], F32); xn3T = dscr("xn3T", [1024, NBO * 128], BF16)

    with ExitStack() as top:
        P = Prog(nc, top)

        def SB(st, name, shape, dt):
            return Buf(P, st.enter_context(nc.sbuf_tensor(name, list(shape), dt)))

        def PS(st, name, shape, dt):
            return Buf(P, st.enter_context(nc.psum_tensor(name, list(shape), dt)))

        ident = SB(top, "ident", [128, 128], BF16)
        cneg = SB(top, "cneg", [128, NKB, 8], F32)
        valid_sb = SB(top, "valid_sb", [128, NKB], F32)
        posf = SB(top, "posf", [128, NKB], F32)
        gates = SB(top, "gates", [128, NBO, 8], F32)
        P.dma('gpsimd', ident[:], c_ident[:, :], W=[ident])
        P.dma('sync', valid_sb[:], valid[:, :], W=[valid_sb])

        def rope_tables(st, name, inv_dram, nf, b0, nb):
            inv = SB(st, name + "_inv", [128, nf], F32)
            P.dma('sync', inv[:], inv_dram[:, :], W=[inv])
            ang = SB(st, name + "_ang", [128, nb, nf], F32)
            tmp = SB(st, name + "_tmp", [128, nb, nf], F32)
            ct = SB(st, name + "_c", [128, nb, nf], F32); stt = SB(st, name + "_s", [128, nb, nf], F32)
            P.op('vector', lambda e: e.tensor_tensor(out=ang[:], in0=posf[:, b0:b0 + nb].unsqueeze(2).broadcast_to([128, nb, nf]),
                                                     in1=inv[:].unsqueeze(1).broadcast_to([128, nb, nf]), op=ALU.mult), R=[posf, inv], W=[ang])
            for dst, off in ((stt, 0.0), (ct, 0.25)):
                P.op('vector', lambda e, off=off: e.tensor_scalar(out=dst[:], in0=ang[:], scalar1=1.0 / (2 * math.pi), scalar2=off, op0=ALU.mult, op1=ALU.add), R=[ang], W=[dst])
                P.op('vector', lambda e: e.tensor_scalar(out=tmp[:], in0=dst[:], scalar1=MAGIC, scalar2=-MAGIC, op0=ALU.add, op1=ALU.add), R=[dst], W=[tmp])
                P.op('vector', lambda e: e.tensor_tensor(out=dst[:], in0=dst[:], in1=tmp[:], op=ALU.subtract), R=[tmp, dst], W=[dst])
                P.op('scalar', lambda e: e.activation(out=dst[:], in_=dst[:], func=AF.Sin, scale=2 * math.pi), R=[dst], W=[dst])
            return ct, stt

        def rmsnorm_T(st_bufs, src_ap, src_buf, n, gain, dstT_ap, dstT_buf, pA, rstd_out=None):
            junk, ssq, xn = st_bufs
            nchunk = n // 128
            P.op('scalar', lambda e: e.activation(out=junk[:, 0:n], in_=src_ap, func=AF.Square, accum_out=ssq[:, 0:1]), R=[src_buf], W=[junk, ssq])
            P.op('scalar', lambda e: e.activation(out=ssq[:, 1:2], in_=ssq[:, 0:1], func=AF.Sqrt, scale=1.0 / n, bias=EPS), R=[ssq], W=[ssq])
            P.op('vector', lambda e: e.reciprocal(out=ssq[:, 1:2], in_=ssq[:, 1:2]), R=[ssq], W=[ssq])
            if rstd_out is not None:
                P.op('vector', lambda e: e.tensor_copy(out=rstd_out[0], in_=ssq[:, 1:2]), R=[ssq], W=[rstd_out[1]])
            P.op('vector', lambda e: e.tensor_scalar(out=xn[:, 0:n], in0=src_ap, scalar1=ssq[:, 1:2], scalar2=None, op0=ALU.mult), R=[src_buf, ssq], W=[xn])
            P.mm([lambda e, c=c: e.transpose(out=pA[:, c, :], in_=xn[:, c * 128:(c + 1) * 128], identity=ident[:]) for c in range(nchunk)], R=[xn], W=[pA])
            P.op('vector', lambda e: e.tensor_tensor(out=dstT_ap, in0=pA[:, 0:nchunk, :], in1=gain[:, 0:nchunk].unsqueeze(2).broadcast_to([128, nchunk, 128]), op=ALU.mult), R=[pA], W=[dstT_buf])

        with ExitStack() as ph:
            posi = SB(ph, "posi", [128, NKB], I32)
            P.dma('sync', posi[:], pos[:, :], W=[posi])
            P.op('vector', lambda e: e.tensor_copy(out=posf[:], in_=posi[:]), R=[posi], W=[posf])
            cm, sm = rope_tables(ph, "rm", c_invm, 16, 0, NKB)
            win = SB(ph, "win", [128, 8, 2216], BF16)
            src = w_in0.rearrange("(c p) n -> p c n", p=128)
            for (d0, s0, n) in ((0, 0, 1536), (1536, 1544, 384), (1920, 1928, 256), (2176, 2184, 32), (2208, 1536, 8)):
                P.dma('gpsimd', win[:, :, d0:d0 + n], src[:, :, s0:s0 + n], W=[win])
            wq = SB(ph, "wq", [128, 3, 768], BF16)
            P.dma('gpsimd', wq[:], wqup.rearrange("(c p) n -> p c n", p=128), W=[wq])
            wkv = SB(ph, "wkv", [128, 2, 2, 8, 64], BF16)
            for c in range(2):
                for t in range(2):
                    P.dma('gpsimd', wkv[:, c, t, :, :], wkvup[c * 128:(c + 1) * 128, :].rearrange("p (h t d) -> p t h d", h=8, t=2)[:, t, :, :], W=[wkv])
            gm = SB(ph, "gm", [128, 8], F32); P.dma('sync', gm[:], g0mix[:, :], W=[gm])
            gq = SB(ph, "gq", [128, 5], F32)
            P.dma('sync', gq[:, 0:3], qng[:, :], W=[gq]); P.dma('sync', gq[:, 3:5], kvng[:, :], W=[gq])
            bfb = SB(ph, "bfb", [128, 8], F32); P.dma('sync', bfb[:], bfor.partition_broadcast(128), W=[bfb])
            flog = SB(ph, "flog", [128, NKB, 8], F32)

            xt = [SB(ph, f"xt{i}", [128, 1024], F32) for i in range(2)]
            junk = SB(ph, "junk", [128, 1024], F32); ssq = SB(ph, "ssq", [128, 2], F32); xn = SB(ph, "xn", [128, 1024], BF16)
            junk2 = SB(ph, "junk2", [128, 384], F32); ssq2 = SB(ph, "ssq2", [128, 2], F32); ssq3 = SB(ph, "ssq3", [128, 2], F32)
            cn = SB(ph, "cn", [128, 640], BF16)
            xnT = SB(ph, "xnT", [128, 8, 128], BF16); cT = SB(ph, "cT", [128, 5, 128], BF16)
            qk_sb = [SB(ph, f"qk_sb{i}", [128, 8, 128], BF16) for i in range(2)]
            fv_sb = [SB(ph, f"fv_sb{i}", [128, 512], BF16) for i in range(2)]
            vm_sb = [SB(ph, f"vm_sb{i}", [128, 512], BF16) for i in range(2)]
            kn_sb = [SB(ph, f"kn_sb{i}", [128, 4, 128], BF16) for i in range(2)]
            misc = SB(ph, "misc", [128, 40], F32)
            kpe = SB(ph, "kpe", [128, 32], BF16); kpeT = [SB(ph, f"kpeT{i}", [32, 128], BF16) for i in range(2)]
            rt = [SB(ph, f"rt{i}", [128, 8, 16], F32) for i in range(4)]
            qpe = SB(ph, "qpe", [128, 8, 32], F32)
            qm_sb = SB(ph, "qm_sb", [128, 8, 96], BF16); qmT = [SB(ph, f"qmT{i}", [96, 8, 128], BF16) for i in range(2)]
            pA = PS(ph, "pA", [128, 8, 128], BF16)
            pQK = PS(ph, "pQK", [128, 8, 128], F32)
            pTM = [PS(ph, f"pTM{i}", [128, 512], F32) for i in range(3)]
            pX = PS(ph, "pX", [128, 8, 128], BF16)
            pY = PS(ph, "pY", [128, 128], BF16)

            P.barrier()
            for kb in range(NKB):
                isq = kb >= QB0
                qi = kb - QB0
                s = kb % 2
                X = xt[s]
                P.dma('sync', X[:], xk[kb * 128:(kb + 1) * 128, :], W=[X])
                rmsnorm_T((junk, ssq, xn), X[:], X, 1024, gm, xnT[:], xnT, pA)
                items = []
                for grp, base in ((0, 0), (1, 512)):
                    if grp == 0 and not isq: continue
                    for p_ in range(4):
                        for c in range(8):
                            items.append(lambda e, p_=p_, c=c, grp=grp, base=base: e.matmul(
                                pQK[:, grp * 4 + p_, :], lhsT=win[:, c, base + p_ * 128: base + (p_ + 1) * 128], rhs=xnT[:, c, :], start=(c == 0), stop=(c == 7)))
                P.mm(items, R=[xnT], W=[pQK])
                for g, (c0, n) in enumerate(((1024, 512), (1536, 384), (1920, 296))):
                    if g == 1 and not isq: continue
                    P.mm([lambda e, c=c, g=g, c0=c0, n=n: e.matmul(pTM[g][:, 0:n], lhsT=xnT[:, c, :], rhs=win[:, c, c0:c0 + n], start=(c == 0), stop=(c == 7)) for c in range(8)],
                         R=[xnT], W=[pTM[g]])
                QK = qk_sb[s]
                lo = 0 if isq else 4
                P.op('scalar', lambda e, QK=QK, lo=lo: e.activation(out=QK[:, lo:8, :], in_=pQK[:, lo:8, :], func=AF.Copy), R=[pQK], W=[QK])
                for j in range(2):
                    if isq:
                        P.dma('gpsimd', qf[j::2, :, qi * 128:(qi + 1) * 128].rearrange("h d t -> d h t"), QK[j * 64:(j + 1) * 64, 0:4, :], R=[QK])
                    P.dma('gpsimd', kf[j::2, :, kb * 128:(kb + 1) * 128].rearrange("h d t -> d h t"), QK[j * 64:(j + 1) * 64, 4:8, :], R=[QK])
                FV = fv_sb[s]
                P.op('vector', lambda e, FV=FV: e.tensor_copy(out=FV[:], in_=pTM[0][:]), R=[pTM[0]], W=[FV])
                P.dma('gpsimd', vf[kb * 128:(kb + 1) * 128, :], FV[:], R=[FV])
                def small_norm(psrc, pbuf, n, ssb, dst_ap):
                    P.op('scalar', lambda e: e.activation(out=junk2[:, 0:n], in_=psrc, func=AF.Square, accum_out=ssb[:, 0:1]), R=[pbuf], W=[junk2, ssb])
                    P.op('scalar', lambda e: e.activation(out=ssb[:, 1:2], in_=ssb[:, 0:1], func=AF.Sqrt, scale=1.0 / n, bias=EPS), R=[ssb], W=[ssb])
                    P.op('vector', lambda e: e.reciprocal(out=ssb[:, 1:2], in_=ssb[:, 1:2]), R=[ssb], W=[ssb])
                    P.op('vector', lambda e: e.tensor_scalar(out=dst_ap, in0=psrc, scalar1=ssb[:, 1:2], scalar2=None, op0=ALU.mult), R=[pbuf, ssb], W=[cn])
                if isq:
                    small_norm(pTM[1][:, 0:384], pTM[1], 384, ssq2, cn[:, 0:384])
                small_norm(pTM[2][:, 0:256], pTM[2], 256, ssq3, cn[:, 384:640])
                P.op('scalar', lambda e: e.activation(out=misc[:], in_=pTM[2][:, 256:296], func=AF.Copy), R=[pTM[2]], W=[misc])
                P.op('vector', lambda e, kb=kb: e.tensor_tensor(out=flog[:, kb, :], in0=misc[:, 32:40], in1=bfb[:], op=ALU.add), R=[misc], W=[flog])
                cb = cm[:, kb, :]; sb_ = sm[:, kb, :]
                k1 = misc[:, 0:16]; k2 = misc[:, 16:32]
                T = [r[:, 0, :] for r in rt]
                P.op('gpsimd', lambda e: e.tensor_tensor(out=T[0], in0=k1, in1=cb, op=ALU.mult), R=[misc], W=[rt[0]])
                P.op('gpsimd', lambda e: e.tensor_tensor(out=T[1], in0=k2, in1=sb_, op=ALU.mult), R=[misc], W=[rt[1]])
                P.op('gpsimd', lambda e: e.tensor_tensor(out=T[2], in0=k2, in1=cb, op=ALU.mult), R=[misc], W=[rt[2]])
                P.op('gpsimd', lambda e: e.tensor_tensor(out=T[3], in0=k1, in1=sb_, op=ALU.mult), R=[misc], W=[rt[3]])
                P.op('gpsimd', lambda e: e.tensor_tensor(out=kpe[:, 0:16], in0=T[0], in1=T[1], op=ALU.subtract), R=[rt[0], rt[1]], W=[kpe])
                P.op('gpsimd', lambda e: e.tensor_tensor(out=kpe[:, 16:32], in0=T[2], in1=T[3], op=ALU.add), R=[rt[2], rt[3]], W=[kpe])
                c_lo = 0 if isq else 3
                P.mm([lambda e, c=c: e.transpose(out=pA[:, c, :], in_=cn[:, c * 128:(c + 1) * 128], identity=ident[:]) for c in range(c_lo, 5)], R=[cn], W=[pA])
                P.op('vector', lambda e, c_lo=c_lo: e.tensor_tensor(out=cT[:, c_lo:5, :], in0=pA[:, c_lo:5, :], in1=gq[:, c_lo:5].unsqueeze(2).broadcast_to([128, 5 - c_lo, 128]), op=ALU.mult), R=[pA], W=[cT])
                items = []
                for p_ in range(4):
                    for c in range(2):
                        items.append(lambda e, p_=p_, c=c: e.matmul(pTM[0][:, p_ * 128:(p_ + 1) * 128], lhsT=wkv[:, c, 0, 2 * p_:2 * p_ + 2, :], rhs=cT[:, 3 + c, :], start=(c == 0), stop=(c == 1)))
                P.mm(items, R=[cT], W=[pTM[0]])
                P.mm([lambda e, c=c: e.matmul(pTM[2][:], lhsT=cT[:, 3 + c, :], rhs=wkv[:, c, 1, :, :], start=(c == 0), stop=(c == 1)) for c in range(2)], R=[cT], W=[pTM[2]])
                KN = kn_sb[s]
                P.op('scalar', lambda e, KN=KN: e.activation(out=KN[:], in_=pTM[0][:].rearrange("p (a b) -> p a b", a=4), func=AF.Copy), R=[pTM[0]], W=[KN])
                for j in range(2):
                    P.dma('gpsimd', km[j::2, 0:64, kb * 128:(kb + 1) * 128].rearrange("h d t -> d h t"), KN[j * 64:(j + 1) * 64, :, :], R=[KN])
                VM = vm_sb[s]
                P.op('vector', lambda e, VM=VM: e.tensor_copy(out=VM[:], in_=pTM[2][:]), R=[pTM[2]], W=[VM])
                P.dma('gpsimd', vm[kb * 128:(kb + 1) * 128, :], VM[:], R=[VM])
                KT = kpeT[s]
                P.mm([lambda e: e.transpose(out=pY[0:32, :], in_=kpe[:, :], identity=ident[:])], R=[kpe], W=[pY])
                P.op('vector', lambda e, KT=KT: e.tensor_copy(out=KT[:], in_=pY[0:32, :]), R=[pY], W=[KT])
                P.dma('gpsimd', km[:, 64:96, kb * 128:(kb + 1) * 128].rearrange("h d t -> d h t"), KT[:].unsqueeze(1).broadcast_to([32, 8, 128]), R=[KT])
                if isq:
                    pq = pQK[:].rearrange("p a b -> p (a b)")
                    items = []
                    for (c0, n) in ((0, 512), (512, 256)):
                        for c in range(3):
                            items.append(lambda e, c=c, c0=c0, n=n: e.matmul(pq[:, c0:c0 + n], lhsT=cT[:, c, :], rhs=wq[:, c, c0:c0 + n], start=(c == 0), stop=(c == 2)))
                    P.mm(items, R=[cT], W=[pQK])
                    pq3 = pq[:, 0:768].rearrange("p (h d) -> p h d", h=8)
                    P.op('scalar', lambda e: e.activation(out=qm_sb[:, :, 0:64], in_=pq3[:, :, 0:64], func=AF.Copy), R=[pQK], W=[qm_sb])
                    P.op('scalar', lambda e: e.activation(out=qpe[:], in_=pq3[:, :, 64:96], func=AF.Copy), R=[pQK], W=[qpe])
                    cb8 = cm[:, kb, :].unsqueeze(1).broadcast_to([128, 8, 16]); sb8 = sm[:, kb, :].unsqueeze(1).broadcast_to([128, 8, 16])
                    q1 = qpe[:, :, 0:16]; q2 = qpe[:, :, 16:32]
                    P.op('gpsimd', lambda e: e.tensor_tensor(out=rt[0][:], in0=q1, in1=cb8, op=ALU.mult), R=[qpe], W=[rt[0]])
                    P.op('gpsimd', lambda e: e.tensor_tensor(out=rt[1][:], in0=q2, in1=sb8, op=ALU.mult), R=[qpe], W=[rt[1]])
                    P.op('gpsimd', lambda e: e.tensor_tensor(out=rt[2][:], in0=q2, in1=cb8, op=ALU.mult), R=[qpe], W=[rt[2]])
                    P.op('gpsimd', lambda e: e.tensor_tensor(out=rt[3][:], in0=q1, in1=sb8, op=ALU.mult), R=[qpe], W=[rt[3]])
                    P.op('gpsimd', lambda e: e.tensor_tensor(out=qm_sb[:, :, 64:80], in0=rt[0][:], in1=rt[1][:], op=ALU.subtract), R=[rt[0], rt[1]], W=[qm_sb])
                    P.op('gpsimd', lambda e: e.tensor_tensor(out=qm_sb[:, :, 80:96], in0=rt[2][:], in1=rt[3][:], op=ALU.add), R=[rt[2], rt[3]], W=[qm_sb])
                    P.mm([lambda e, h=h: e.transpose(out=pX[0:96, h, :], in_=qm_sb[:, h, :], identity=ident[:]) for h in range(8)], R=[qm_sb], W=[pX])
                    QT = qmT[s]
                    P.op('vector', lambda e, QT=QT: e.tensor_copy(out=QT[:], in_=pX[0:96, :, :]), R=[pX], W=[QT])
                    P.dma('gpsimd', qm[:, :, qi * 128:(qi + 1) * 128].rearrange("h d t -> d h t"), QT[:], R=[QT])

            NC8 = NKB * 8
            U = SB(ph, "U", [128, 128], F32); SU = SB(ph, "SU", [NKB, NKB], F32); eye = SB(ph, "eye", [NKB, NKB], F32)
            P.dma('sync', U[:], c_U[:, :], W=[U]); P.dma('sync', SU[:], c_SU[:, :], W=[SU]); P.dma('sync', eye[:], c_eye[:, :], W=[eye])
            onesf = SB(ph, "onesf", [128, 128], F32)
            P.op('vector', lambda e: e.memset(onesf[:], 1.0), W=[onesf])
            logf = SB(ph, "logf", [128, NKB, 8], F32)
            P.barrier()
            P.op('scalar', lambda e: e.activation(out=logf[:], in_=flog[:], func=AF.Exp, scale=-1.0), R=[flog], W=[logf])
            P.op('scalar', lambda e: e.activation(out=logf[:], in_=logf[:], func=AF.Ln, bias=1.0), R=[logf], W=[logf])
            P.op('vector', lambda e: e.tensor_scalar(out=logf[:], in0=logf[:], scalar1=-1.0, scalar2=None, op0=ALU.mult), R=[logf], W=[logf])
            pc = pTM[0]; pc2 = pTM[1]; pc3 = pTM[2]
            tot = SB(ph, "tot", [NKB, 8], F32); pre = SB(ph, "pre", [NKB, 8], F32); dpre = SB(ph, "dpre", [NKB, NKB, 8], F32)
            P.mm([lambda e, h=h: e.matmul(pc[0:NKB, h:h + 1], lhsT=logf[:, :, h], rhs=onesf[:, 0:1], start=True, stop=True) for h in range(8)], R=[logf], W=[pc])
            P.op('vector', lambda e: e.tensor_copy(out=tot[:], in_=pc[0:NKB, 0:8]), R=[pc], W=[tot])
            P.mm([lambda e: e.matmul(pc2[0:NKB, 0:8], lhsT=SU[:], rhs=tot[:], start=True, stop=True)], R=[tot, SU], W=[pc2])
            P.op('vector', lambda e: e.tensor_copy(out=pre[:], in_=pc2[0:NKB, 0:8]), R=[pc2], W=[pre])
            P.op('vector', lambda e: e.tensor_tensor(out=dpre[:], in0=pre[:].unsqueeze(1).broadcast_to([NKB, NKB, 8]),
                                                     in1=eye[:].unsqueeze(2).broadcast_to([NKB, NKB, 8]), op=ALU.mult), R=[pre, eye], W=[dpre])
            P.mm([lambda e: e.matmul(pc3[:, 0:NC8], lhsT=U[:], rhs=logf[:].rearrange("p a b -> p (a b)"), start=True, stop=False),
                  lambda e: e.matmul(pc3[:, 0:NC8], lhsT=onesf[0:NKB, :], rhs=dpre[:].rearrange("p a b -> p (a b)"), start=False, stop=True)],
                 R=[logf, dpre, U], W=[pc3])
            P.op('scalar', lambda e: e.activation(out=cneg[:].rearrange("p a b -> p (a b)"), in_=pc3[:, 0:NC8], func=AF.Copy, scale=-1.0), R=[pc3], W=[cneg])
            P.barrier(); P.emit()
        if upto <= 1:
            return nc

        with ExitStack() as ph:
            kT = [SB(ph, f"kT{i}", [128, NK], BF16) for i in range(2)]
            vA = [SB(ph, f"vA{i}", [128, NKB, 128], BF16) for i in range(2)]
            qT = [SB(ph, f"qT{i}", [128, NQ], BF16) for i in range(2)]
            pT = [SB(ph, f"pT{i}", [128, 2, 512], BF16) for i in range(3)]
            mneg = SB(ph, "mneg", [128, 128], BF16)
            P.dma('gpsimd', mneg[:], c_mown[:, :], W=[mneg])
            l_lo = SB(ph, "l_lo", [64, 512], F32)
            o_sb = [SB(ph, f"o_sb{i}", [64, 512], BF16) for i in range(2)]
            pS = [PS(ph, f"pS{i}", [128, 2, 512], F32) for i in range(3)]
            pO = [PS(ph, f"pO{i}", [128, 512], F32) for i in range(2)]
            KPAD = 'nopad' not in DBG
            for s in range(2):
                P.op('vector', lambda e, s=s: e.tensor_copy(out=vA[s][:, :, 64:128], in_=valid_sb[:].unsqueeze(2).broadcast_to([128, NKB, 64])), R=[valid_sb], W=[vA[s]])
                if KPAD:
                    P.op('vector', lambda e, s=s: e.memset(kT[s][64:128, :], 0.0), W=[kT[s]])
                    P.op('vector', lambda e, s=s: e.memset(qT[s][64:128, :], 0.0), W=[qT[s]])
                P.op('vector', lambda e, s=s: e.memset(kT[s][64:65, :], 1.0), W=[kT[s]])
            qtiles = [(0, 1)] + [(1 + 4 * i, 4) for i in range((NQB - 1) // 4)]
            cnt = 0
            P.barrier()
            for hh in range(16):
                fox = hh < 8; h = hh % 8
                Dk = 64 if fox else 96
                Dka = 128 if KPAD else (65 if fox else 96)
                if KPAD and hh == 8:
                    for s_ in range(2):
                        P.op('vector', lambda e, s_=s_: e.memset(kT[s_][96:128, :], 0.0), W=[kT[s_]])
                        P.op('vector', lambda e, s_=s_: e.memset(qT[s_][96:128, :], 0.0), W=[qT[s_]])
                s = hh % 2
                KT, VA, QT = kT[s], vA[s], qT[s]
                ksrc, vsrc, qsrc = (kf, vf, qf) if fox else (km, vm, qm)
                P.dma('sync', KT[0:Dk, :], ksrc[h, :, :], W=[KT])
                P.dma('sync', VA[:, :, 0:64], vsrc[:, h * 64:(h + 1) * 64].rearrange("(kb t) d -> t kb d", t=128), W=[VA])
                P.dma('sync', QT[0:Dk, :], qsrc[h, :, :], W=[QT])
                if fox:
                    P.op('vector', lambda e, QT=QT, h=h: e.tensor_scalar(
                        out=QT[64:65, :].rearrange("p (b t) -> p b t", t=128),
                        in0=cneg[0:1, QB0:QB0 + NQB, h].unsqueeze(2).broadcast_to([1, NQB, 128]), scalar1=-8.0, scalar2=None, op0=ALU.mult), R=[cneg], W=[QT])
                scale = 0.125 if fox else 96 ** -0.5
                for (q0, nq) in qtiles:
                    O = pO[cnt % 2]; cnt += 1
                    kq0 = QB0 + q0
                    NQT = nq * 128
                    full = list(range(kq0))
                    units = []
                    if fox or 'nopair' in DBG:
                        units = [[(kb, 0)] for kb in full]
                    else:
                        i_ = 0
                        while i_ + 1 < len(full):
                            units.append([(full[i_], 0), (full[i_ + 1], 0)]); i_ += 2
                        if i_ < len(full):
                            units.append([(full[i_], 0)])
                    units += [[(kq0 + j, j * 128)] for j in range(nq)]
                    pend = None
                    for ui, unit in enumerate(units):
                        S = pS[ui % 3]; PT = pT[ui % 3]
                        items = []
                        for idx, (kb, c0) in enumerate(unit):
                            diag = kb >= kq0
                            items.append(lambda e, kb=kb, c0=c0, S=S, idx=idx, diag=diag: e.matmul(S[:, idx, c0:NQT], lhsT=KT[0:Dka, kb * 128:(kb + 1) * 128], rhs=QT[0:Dka, q0 * 128 + c0:q0 * 128 + NQT], start=True, stop=not diag))
                            if diag:
                                items.append(lambda e, c0=c0, S=S, idx=idx: e.matmul(S[:, idx, c0:c0 + 128], lhsT=ident[:], rhs=mneg[:], start=False, stop=True))
                        P.mm(items, R=[KT, QT], W=[S])
                        if pend is not None:
                            pend()
                        if len(unit) == 2:
                            P.op('scalar', lambda e, S=S, PT=PT: e.activation(out=PT[:, :, 0:NQT], in_=S[:, :, 0:NQT], func=AF.Exp, bias=0.0, scale=scale), R=[S], W=[PT])
                        else:
                            kb, c0 = unit[0]
                            bias = cneg[:, kb, h:h + 1] if fox else 0.0
                            P.op('scalar', lambda e, S=S, PT=PT, c0=c0, bias=bias: e.activation(out=PT[:, 0, c0:NQT], in_=S[:, 0, c0:NQT], func=AF.Exp, bias=bias, scale=scale), R=[S], W=[PT])
                        ufirst = ui == 0; ulast = ui == len(units) - 1
                        def pv(unit=unit, PT=PT, ufirst=ufirst, ulast=ulast, O=O):
                            its = []
                            for idx, (kb, c0) in enumerate(unit):
                                its.append(lambda e, kb=kb, c0=c0, idx=idx: e.matmul(O[:, c0:NQT], lhsT=VA[:, kb, :], rhs=PT[:, idx, c0:NQT], start=(ufirst and idx == 0), stop=(ulast and idx == len(unit) - 1)))
                            P.mm(its, R=[PT, VA], W=[O])
                        pend = pv
                    pend()
                    OS = o_sb[cnt % 2]
                    P.op('vector', lambda e, O=O: e.tensor_scalar(out=l_lo[:, 0:NQT], in0=O[64:128, 0:NQT], scalar1=1e-30, scalar2=None, op0=ALU.add), R=[O], W=[l_lo])
                    P.op('vector', lambda e: e.reciprocal(out=l_lo[:, 0:NQT], in_=l_lo[:, 0:NQT]), R=[l_lo], W=[l_lo])
                    P.op('vector', lambda e, O=O, OS=OS: e.tensor_tensor(out=OS[:, 0:NQT], in0=O[0:64, 0:NQT], in1=l_lo[:, 0:NQT], op=ALU.mult), R=[O, l_lo], W=[OS])
                    row0 = (0 if fox else 512) + h * 64
                    P.dma('gpsimd', mixT[row0:row0 + 64, q0 * 128:q0 * 128 + NQT], OS[:, 0:NQT], R=[OS])
            P.barrier(); P.emit()
        if upto <= 2:
            return nc

        def wout_phase(ph, name, wout_d, mix_d, res_fn, nblk, gain_d, h_d, xT_d, router=None):
            wo = SB(ph, name + "wo", [128, 8, 1024], BF16)
            P.dma('gpsimd', wo[:], wout_d.rearrange("(c p) n -> p c n", p=128), W=[wo])
            g = SB(ph, name + "g", [128, 8], F32); P.dma('sync', g[:], gain_d[:, :], W=[g])
            mx = [SB(ph, f"{name}mx{i}", [128, 8, 128], BF16) for i in range(2)]
            xr = [SB(ph, f"{name}xr{i}", [128, 1024], F32) for i in range(2)]
            ht = [SB(ph, f"{name}ht{i}", [128, 1024], F32) for i in range(2)]
            xT = [SB(ph, f"{name}xT{i}", [128, 8, 128], BF16) for i in range(2)]
            junk = SB(ph, name + "junk", [128, 1024], F32); ssq = SB(ph, name + "ssq", [128, 2], F32); xn = SB(ph, name + "xn", [128, 1024], BF16)
            pW = [PS(ph, f"{name}pW{i}", [128, 512], F32) for i in range(2)]
            pA = PS(ph, name + "pA", [128, 8, 128], BF16)
            if router is not None:
                wr = SB(ph, name + "wr", [128, 8, 1024], F32)
                P.dma('sync', wr[:].rearrange("p a b -> p (a b)"), wrT.rearrange("a b -> (a b)").partition_broadcast(128), W=[wr])
                grow = SB(ph, name + "grow", [128, 1024], F32)
                P.dma('sync', grow[:], g1ffn_row.partition_broadcast(128), W=[grow])
                P.op('gpsimd', lambda e: e.tensor_tensor(out=wr[:], in0=wr[:], in1=grow[:].unsqueeze(1).broadcast_to([128, 8, 1024]), op=ALU.mult), R=[wr, grow], W=[wr])
                lg = SB(ph, name + "lg", [128, 8], F32); l2 = SB(ph, name + "l2", [128, 8], F32)
                mk1 = SB(ph, name + "mk1", [128, 8], F32); mk2 = SB(ph, name + "mk2", [128, 8], F32)
                sc = SB(ph, name + "sc", [128, 8], F32); rs = SB(ph, name + "rs", [128, 1], F32)
                rjs = [SB(ph, f"{name}rj{i}", [128, 1024], F32) for i in range(4)]
            P.barrier()
            for j in range(nblk):
                s = j % 2
                MX, XR, HT, XT = mx[s], xr[s], ht[s], xT[s]
                P.dma('sync', MX[:], mix_d[:, j * 128:(j + 1) * 128].rearrange("(c p) t -> p c t", p=128), W=[MX])
                P.dma('sync', XR[:], res_fn(j), W=[XR])
                for half in range(2):
                    P.mm([lambda e, c=c, half=half: e.matmul(pW[half][:], lhsT=MX[:, c, :], rhs=wo[:, c, half * 512:(half + 1) * 512], start=(c == 0), stop=(c == 7)) for c in range(8)], R=[MX], W=[pW[half]])
                    P.op('vector', lambda e, half=half: e.tensor_tensor(out=HT[:, half * 512:(half + 1) * 512], in0=pW[half][:], in1=XR[:, half * 512:(half + 1) * 512], op=ALU.add), R=[pW[half], XR], W=[HT])
                P.dma('gpsimd', h_d[j * 128:(j + 1) * 128, :], HT[:], R=[HT])
                rmsnorm_T((junk, ssq, xn), HT[:], HT, 1024, g, XT[:], XT, pA, rstd_out=(rs[:, 0:1], rs) if router is not None else None)
                P.dma('gpsimd', xT_d[:, j * 128:(j + 1) * 128].rearrange("(c p) t -> p c t", p=128), XT[:], R=[XT])
                if router is not None:
                    for e_ in range(8):
                        RJ = rjs[e_ % 4]
                        P.op('vector' if e_ % 2 == 0 else 'gpsimd', lambda e, e_=e_, RJ=RJ: e.tensor_tensor(out=RJ[:], in0=HT[:], in1=wr[:, e_, :], op=ALU.mult), R=[HT, wr], W=[RJ])
                        P.op('scalar', lambda e, e_=e_, RJ=RJ: e.activation(out=RJ[:], in_=RJ[:], func=AF.Copy, accum_out=lg[:, e_:e_ + 1]), R=[RJ], W=[RJ, lg])
                    P.op('vector', lambda e: e.tensor_scalar(out=lg[:], in0=lg[:], scalar1=rs[:, 0:1], scalar2=None, op0=ALU.mult), R=[lg, rs], W=[lg])
                    P.op('vector', lambda e: e.reduce_max(out=sc[:, 0:1], in_=lg[:], axis=mybir.AxisListType.X), R=[lg], W=[sc])
                    P.op('vector', lambda e: e.tensor_scalar(out=mk1[:], in0=lg[:], scalar1=sc[:, 0:1], scalar2=None, op0=ALU.is_equal), R=[lg, sc], W=[mk1])
                    P.op('vector', lambda e: e.scalar_tensor_tensor(out=l2[:], in0=mk1[:], scalar=-1e30, in1=lg[:], op0=ALU.mult, op1=ALU.add), R=[mk1, lg], W=[l2])
                    P.op('vector', lambda e: e.reduce_max(out=sc[:, 1:2], in_=l2[:], axis=mybir.AxisListType.X), R=[l2], W=[sc])
                    P.op('vector', lambda e: e.tensor_scalar(out=mk2[:], in0=l2[:], scalar1=sc[:, 1:2], scalar2=None, op0=ALU.is_equal), R=[l2, sc], W=[mk2])
                    P.op('vector', lambda e: e.tensor_tensor(out=sc[:, 2:3], in0=sc[:, 1:2], in1=sc[:, 0:1], op=ALU.subtract), R=[sc], W=[sc])
                    P.op('scalar', lambda e: e.activation(out=sc[:, 3:4], in_=sc[:, 2:3], func=AF.Exp), R=[sc], W=[sc])
                    P.op('vector', lambda e: e.tensor_scalar(out=sc[:, 4:5], in0=sc[:, 3:4], scalar1=1.0, scalar2=None, op0=ALU.add), R=[sc], W=[sc])
                    P.op('vector', lambda e: e.reciprocal(out=sc[:, 4:5], in_=sc[:, 4:5]), R=[sc], W=[sc])
                    P.op('vector', lambda e: e.tensor_tensor(out=sc[:, 5:6], in0=sc[:, 3:4], in1=sc[:, 4:5], op=ALU.mult), R=[sc], W=[sc])
                    P.op('vector', lambda e: e.tensor_scalar(out=mk1[:], in0=mk1[:], scalar1=sc[:, 4:5], scalar2=None, op0=ALU.mult), R=[mk1, sc], W=[mk1])
                    P.op('vector', lambda e, j=j: e.scalar_tensor_tensor(out=gates[:, j, :], in0=mk2[:], scalar=sc[:, 5:6], in1=mk1[:], op0=ALU.mult, op1=ALU.add), R=[mk2, mk1, sc], W=[gates])

        def ffn_phase(ph, name, tiles, h_src, xT_src, experts, finish):
            NBT = max(nb for _, nb in tiles); NT = NBT * 128
            htile = SB(ph, name + "ht", [128, NBT, 1024], F32)
            xT = SB(ph, name + "xT", [128, 8, NT], BF16)
            actT = SB(ph, name + "act", [128, NF, NT], BF16)
            wdq = [SB(ph, f"{name}wd{i}", [128, 7, 1024], BF16) for i in range(4)]
            wgu = [SB(ph, f"{name}wgu{i}", [128, 8, 2, 128], BF16) for i in range(3)]
            sg = [SB(ph, f"{name}sg{i}", [128, 512], F32) for i in range(2)]
            pG = [PS(ph, f"{name}pG{i}", [128, 512], F32) for i in range(2)]
            pU = [PS(ph, f"{name}pU{i}", [128, 512], F32) for i in range(2)]
            pD = [PS(ph, f"{name}pD{i}", [128, 512], F32) for i in range(2)]
            wcnt = 0; ccnt = 0; dcnt = 0
            for (b0, nb) in tiles:
                N = nb * 128
                chunks = [(c0, min(512, N - c0)) for c0 in range(0, N, 512)]
                P.dma('sync', xT[:, :, 0:N], xT_src[:, b0 * 128:(b0 + nb) * 128].rearrange("(c p) n -> p c n", p=128), W=[xT])
                P.dma('sync', htile[:, 0:nb, :], h_src[b0 * 128:(b0 + nb) * 128, :].rearrange("(j t) d -> t j d", t=128), W=[htile])
                for (wg, wu, wd, gi) in experts:
                    for f in range(NF):
                        WGU = wgu[wcnt % 3]; wcnt += 1
                        P.dma('gpsimd', WGU[:, :, 0, :], wg[:, f * 128:(f + 1) * 128].rearrange("(c p) n -> p c n", p=128), W=[WGU])
                        P.dma('gpsimd', WGU[:, :, 1, :], wu[:, f * 128:(f + 1) * 128].rearrange("(c p) n -> p c n", p=128), W=[WGU])
                        if f % 7 == 3:
                            k_ = f // 7
                            P.dma('gpsimd', wdq[k_][:], wd[k_ * 896:(k_ + 1) * 896, :].rearrange("(f p) n -> p f n", p=128), W=[wdq[k_]])
                        for (c0, n) in chunks:
                            G = pG[ccnt % 2]; Uu = pU[ccnt % 2]; SG = sg[ccnt % 2]; ccnt += 1
                            P.mm([lambda e, c=c, G=G, c0=c0, n=n, WGU=WGU: e.matmul(G[:, 0:n], lhsT=WGU[:, c, 0, :], rhs=xT[:, c, c0:c0 + n], start=(c == 0), stop=(c == 7)) for c in range(8)], R=[WGU, xT], W=[G])
                            P.mm([lambda e, c=c, Uu=Uu, c0=c0, n=n, WGU=WGU: e.matmul(Uu[:, 0:n], lhsT=WGU[:, c, 1, :], rhs=xT[:, c, c0:c0 + n], start=(c == 0), stop=(c == 7)) for c in range(8)], R=[WGU, xT], W=[Uu])
                            P.op('scalar', lambda e, G=G, SG=SG, n=n: e.activation(out=SG[:, 0:n], in_=G[:, 0:n], func=AF.Silu), R=[G], W=[SG])
                            P.op('vector', lambda e, Uu=Uu, SG=SG, n=n, f=f, c0=c0: e.tensor_tensor(out=actT[:, f, c0:c0 + n], in0=Uu[:, 0:n], in1=SG[:, 0:n], op=ALU.mult), R=[Uu, SG], W=[actT])
                    for j in range(nb):
                        for half in range(2):
                            D = pD[dcnt % 2]; dcnt += 1
                            P.mm([lambda e, f=f, j=j, half=half, D=D: e.matmul(D[:], lhsT=actT[:, f, j * 128:(j + 1) * 128], rhs=wdq[f // 7][:, f % 7, half * 512:(half + 1) * 512], start=(f == 0), stop=(f == NF - 1)) for f in range(NF)],
                                 R=[actT] + wdq, W=[D])
                            hs = htile[:, j, half * 512:(half + 1) * 512]
                            if gi is None:
                                P.op('vector', lambda e, D=D, hs=hs: e.tensor_tensor(out=hs, in0=D[:], in1=hs, op=ALU.add), R=[D, htile], W=[htile])
                            else:
                                gcol = gates[:, b0 + j, gi:gi + 1]
                                P.op('vector', lambda e, D=D, hs=hs, gcol=gcol: e.scalar_tensor_tensor(out=hs, in0=D[:], scalar=gcol, in1=hs, op0=ALU.mult, op1=ALU.add), R=[D, htile, gates], W=[htile])
                finish(htile, b0, nb)

        def ffn_tiles(first_single, nblk):
            t = []
            b = 0
            if first_single:
                t.append((0, 1)); b = 1
            while b < nblk:
                nb = min(8, nblk - b); t.append((b, nb)); b += nb
            return t

        with ExitStack() as ph:
            wout_phase(ph, "a", wout0, mixT, lambda j: xk[(QB0 + j) * 128:(QB0 + j + 1) * 128, :], NQB, g0ffn, h1, xn2T)
            P.barrier(); P.emit()
        if upto <= 3:
            return nc
        with ExitStack() as ph:
            def fin0(htile, b0, nb):
                P.dma('gpsimd', h2[b0 * 128:(b0 + nb) * 128, :].rearrange("(j t) d -> t j d", t=128), htile[:, 0:nb, :], R=[htile])
            ffn_phase(ph, "b", ffn_tiles(True, NQB), h1, xn2T, [(wg0, wu0, wd0, None)], fin0)
            P.barrier(); P.emit()
        if upto <= 4:
            return nc

        with ExitStack() as ph:
            cs, ss_ = rope_tables(ph, "rs", c_invs, 32, QB0, NQB)
            win = SB(ph, "win1", [128, 8, 2304], BF16)
            srcw = w_in1.rearrange("(c p) n -> p c n", p=128)
            for c0 in range(0, 2304, 768):
                P.dma('gpsimd', win[:, :, c0:c0 + 768], srcw[:, :, c0:c0 + 768], W=[win])
            gm = SB(ph, "gm1", [128, 8], F32); P.dma('sync', gm[:], g1mix[:, :], W=[gm])
            es = SB(ph, "es", [128, 8], F32); P.dma('sync', es[:], sinks.partition_broadcast(128), W=[es])
            P.op('scalar', lambda e: e.activation(out=es[:], in_=es[:], func=AF.Exp), R=[es], W=[es])
            cw = SB(ph, "cw", [128, 3, 512], F32)
            P.dma('sync', cw[:].rearrange("p a b -> p (a b)"), convw.rearrange("a b -> (a b)").partition_broadcast(128), W=[cw])
            sh = SB(ph, "sh", [128, 4, 128], BF16)
            for i in range(4):
                P.dma('gpsimd', sh[:, i, :], c_sh[i, :, :], W=[sh])
            mk = SB(ph, "mk", [128, 2, 4, 128], BF16)
            for r in range(4):
                P.dma('gpsimd', mk[:, 0, r, :], c_mprev[:, :], W=[mk]); P.dma('gpsimd', mk[:, 1, r, :], c_mown[:, :], W=[mk])
            xt = [SB(ph, f"x1t{i}", [128, 1024], F32) for i in range(2)]
            junk = SB(ph, "junk1", [128, 1024], F32); ssq = SB(ph, "ssq1", [128, 2], F32); xn = SB(ph, "xn1", [128, 1024], BF16)
            xnT = SB(ph, "xnT1", [128, 8, 128], BF16)
            qkf = SB(ph, "qkf", [128, 10, 64], F32)
            qkr = SB(ph, "qkr", [128, 10, 64], BF16)
            rr = [SB(ph, f"rr{i}", [128, 10, 32], F32) for i in range(4)]
            qkT = SB(ph, "qkT", [64, 8, 128], BF16)
            kTd = [SB(ph, f"kTd{i}", [64, 2, 128], BF16) for i in range(2)]
            vAg = [SB(ph, f"vAg{i}", [128, 2, 128], BF16) for i in range(2)]
            pTs = [SB(ph, f"pTs{i}", [128, 512], BF16) for i in range(2)]
            l_lo = SB(ph, "l_lo1", [64, 4, 128], F32); o_sb = [SB(ph, f"o1sb{i}", [64, 4, 128], BF16) for i in range(2)]
            gcs = SB(ph, "gcs", [128, 512], F32); u = SB(ph, "uconv", [128, 512], F32)
            uw = [SB(ph, f"uw{i}", [128, 3, 512], BF16) for i in range(2)]
            oc = SB(ph, "oc", [128, 512], BF16); ocT = [SB(ph, f"ocT{i}", [128, 4, 128], BF16) for i in range(2)]
            pA = PS(ph, "pA1", [128, 8, 128], BF16)
            pP = [PS(ph, f"pP{i}", [128, 512], F32) for i in range(5)]
            pS = PS(ph, "pSw", [128, 512], F32); pO = PS(ph, "pOw", [128, 512], F32)
            groups = ((0, 512), (512, 256), (768, 512), (1280, 512), (1792, 512))
            P.barrier()
            for qi in range(NQB):
                kb = QB0 + qi
                s = qi % 2; sp = 1 - s
                X = xt[s]
                P.dma('sync', X[:], h2[qi * 128:(qi + 1) * 128, :], W=[X])
                rmsnorm_T((junk, ssq, xn), X[:], X, 1024, gm, xnT[:], xnT, pA)
                for g, (c0, n) in enumerate(groups):
                    if qi == 0 and g in (0, 2): continue
                    P.mm([lambda e, c=c, g=g, c0=c0, n=n: e.matmul(pP[g][:, 0:n], lhsT=xnT[:, c, :], rhs=win[:, c, c0:c0 + n], start=(c == 0), stop=(c == 7)) for c in range(8)], R=[xnT], W=[pP[g]])
                if qi > 0:
                    P.op('scalar', lambda e: e.activation(out=qkf[:, 0:8, :], in_=pP[0][:].rearrange("p (h d) -> p h d", h=8), func=AF.Copy), R=[pP[0]], W=[qkf])
                P.op('scalar', lambda e: e.activation(out=qkf[:, 8:10, :], in_=pP[1][:, 0:128].rearrange("p (h d) -> p h d", h=2), func=AF.Copy), R=[pP[1]], W=[qkf])
                lo = 0 if qi > 0 else 8
                nh = 10 - lo
                cbb = cs[:, qi, :].unsqueeze(1).broadcast_to([128, nh, 32]); sbb = ss_[:, qi, :].unsqueeze(1).broadcast_to([128, nh, 32])
                x1 = qkf[:, lo:10, 0:32]; x2 = qkf[:, lo:10, 32:64]
                P.op('gpsimd', lambda e: e.tensor_tensor(out=rr[0][:, lo:10, :], in0=x1, in1=cbb, op=ALU.mult), R=[qkf], W=[rr[0]])
                P.op('gpsimd', lambda e: e.tensor_tensor(out=rr[1][:, lo:10, :], in0=x2, in1=sbb, op=ALU.mult), R=[qkf], W=[rr[1]])
                P.op('vector', lambda e: e.tensor_tensor(out=rr[2][:, lo:10, :], in0=x2, in1=cbb, op=ALU.mult), R=[qkf], W=[rr[2]])
                P.op('vector', lambda e: e.tensor_tensor(out=rr[3][:, lo:10, :], in0=x1, in1=sbb, op=ALU.mult), R=[qkf], W=[rr[3]])
                P.op('gpsimd', lambda e: e.tensor_tensor(out=qkr[:, lo:10, 0:32], in0=rr[0][:, lo:10, :], in1=rr[1][:, lo:10, :], op=ALU.subtract), R=[rr[0], rr[1]], W=[qkr])
                P.op('vector', lambda e: e.tensor_tensor(out=qkr[:, lo:10, 32:64], in0=rr[2][:, lo:10, :], in1=rr[3][:, lo:10, :], op=ALU.add), R=[rr[2], rr[3]], W=[qkr])
                KD = kTd[s]; VG = vAg[s]
                P.mm([lambda e, c=c: e.transpose(out=pA[0:64, c, :], in_=qkr[:, 8 + c, :], identity=ident[:]) for c in range(2)], R=[qkr], W=[pA])
                P.op('vector', lambda e, KD=KD: e.tensor_copy(out=KD[:], in_=pA[0:64, 0:2, :]), R=[pA], W=[KD])
                if qi > 0:
                    P.mm([lambda e, c=c: e.transpose(out=pA[0:64, c, :], in_=qkr[:, c, :], identity=ident[:]) for c in range(8)], R=[qkr], W=[pA])
                    P.op('vector', lambda e: e.tensor_copy(out=qkT[:], in_=pA[0:64, :, :]), R=[pA], W=[qkT])
                P.op('scalar', lambda e, VG=VG: e.activation(out=VG[:, :, 0:64], in_=pP[1][:, 128:256].rearrange("p (g d) -> p g d", g=2), func=AF.Copy), R=[pP[1]], W=[VG])
                P.op('vector', lambda e, VG=VG, kb=kb: e.tensor_copy(out=VG[:, :, 64:128], in_=valid_sb[:, kb:kb + 1].unsqueeze(2).broadcast_to([128, 2, 64])), R=[valid_sb], W=[VG])
                if qi > 0 and 'noswa' not in DBG:
                    for g in range(2):
                        for which, (KDx, VGx) in enumerate(((kTd[sp], vAg[sp]), (KD, VG))):
                            PT = pTs[which]
                            items = [lambda e, KDx=KDx: e.matmul(pS[:], lhsT=KDx[:, g, :], rhs=qkT[:, 4 * g:4 * g + 4, :], start=True, stop=False)]
                            items.append(lambda e, which=which: e.matmul(pS[:], lhsT=ident[:], rhs=mk[:, which, :, :], start=False, stop=True))
                            P.mm(items, R=[KDx, qkT], W=[pS])
                            P.op('scalar', lambda e, PT=PT: e.activation(out=PT[:], in_=pS[:], func=AF.Exp, scale=0.125), R=[pS], W=[PT])
                            P.mm([lambda e, which=which, VGx=VGx, PT=PT: e.matmul(pO[:], lhsT=VGx[:, g, :], rhs=PT[:], start=(which == 0), stop=(which == 1))], R=[PT, VGx], W=[pO])
                        OS = o_sb[g]
                        P.op('vector', lambda e: e.tensor_copy(out=l_lo[:], in_=pO[64:128, :].rearrange("p (a b) -> p a b", a=4)), R=[pO], W=[l_lo])
                        P.op('vector', lambda e, g=g: e.tensor_tensor(out=l_lo[:], in0=l_lo[:], in1=es[0:64, 4 * g:4 * g + 4].unsqueeze(2).broadcast_to([64, 4, 128]), op=ALU.add), R=[l_lo, es], W=[l_lo])
                        P.op('vector', lambda e: e.reciprocal(out=l_lo[:], in_=l_lo[:]), R=[l_lo], W=[l_lo])
                        P.op('vector', lambda e, OS=OS: e.tensor_tensor(out=OS[:], in0=pO[0:64, :].rearrange("p (a b) -> p a b", a=4), in1=l_lo[:], op=ALU.mult), R=[pO, l_lo], W=[OS])
                        P.dma('gpsimd', mix1T[g * 256:(g + 1) * 256, (qi - 1) * 128:qi * 128].rearrange("(a d) q -> d a q", d=64), OS[:], R=[OS])
                UW = uw[s]; UWp = uw[sp]
                if 'noconv' in DBG: continue
                P.op('scalar', lambda e: e.activation(out=gcs[:], in_=pP[3][:], func=AF.Copy), R=[pP[3]], W=[gcs])
                P.op('vector', lambda e: e.tensor_tensor(out=u[:], in0=pP[4][:], in1=gcs[:], op=ALU.mult), R=[pP[4], gcs], W=[u])
                P.op('gpsimd', lambda e, UW=UW: e.tensor_tensor(out=UW[:], in0=u[:].unsqueeze(1).broadcast_to([128, 3, 512]), in1=cw[:], op=ALU.mult), R=[u, cw], W=[UW])
                if qi > 0:
                    P.mm([lambda e: e.matmul(pP[0][:], lhsT=sh[:, 1, :], rhs=UW[:, 0, :], start=True, stop=False),
                          lambda e: e.matmul(pP[0][:], lhsT=sh[:, 0, :], rhs=UW[:, 1, :], start=False, stop=False),
                          lambda e: e.matmul(pP[0][:], lhsT=ident[:], rhs=UW[:, 2, :], start=False, stop=False),
                          lambda e: e.matmul(pP[0][:], lhsT=sh[:, 3, :], rhs=UWp[:, 0, :], start=False, stop=False),
                          lambda e: e.matmul(pP[0][:], lhsT=sh[:, 2, :], rhs=UWp[:, 1, :], start=False, stop=True)], R=[UW, UWp], W=[pP[0]])
                    P.op('scalar', lambda e: e.activation(out=gcs[:], in_=pP[2][:], func=AF.Copy), R=[pP[2]], W=[gcs])
                    P.op('vector', lambda e: e.tensor_tensor(out=oc[:], in0=pP[0][:], in1=gcs[:], op=ALU.mult), R=[pP[0], gcs], W=[oc])
                    P.mm([lambda e, c=c: e.transpose(out=pA[:, c, :], in_=oc[:, c * 128:(c + 1) * 128], identity=ident[:]) for c in range(4)], R=[oc], W=[pA])
                    OT = ocT[s]
                    P.op('vector', lambda e, OT=OT: e.tensor_copy(out=OT[:], in_=pA[:, 0:4, :]), R=[pA], W=[OT])
                    P.dma('gpsimd', mix1T[512:1024, (qi - 1) * 128:qi * 128].rearrange("(c p) t -> p c t", p=128), OT[:], R=[OT])
            P.barrier(); P.emit()
        if upto <= 5:
            return nc

        with ExitStack() as ph:
            wout_phase(ph, "c", wout1, mix1T, lambda j: h2[(j + 1) * 128:(j + 2) * 128, :], NBO, g1ffn, h3, xn3T, router=True)
            P.barrier(); P.emit()
        if upto <= 6:
            return nc
        with ExitStack() as ph:
            gf = SB(ph, "gf", [128, 1024], F32)
            P.dma('sync', gf[:], gfin_row.partition_broadcast(128), W=[gf])
            junk = SB(ph, "junkf", [128, 1024], F32); ssq = SB(ph, "ssqf", [128, 2], F32)
            P.barrier()

            def fin1(htile, b0, nb):
                for j in range(nb):
                    hs = htile[:, j, :]
                    P.op('scalar', lambda e, hs=hs: e.activation(out=junk[:], in_=hs, func=AF.Square, accum_out=ssq[:, 0:1]), R=[htile], W=[junk, ssq])
                    P.op('scalar', lambda e: e.activation(out=ssq[:, 1:2], in_=ssq[:, 0:1], func=AF.Sqrt, scale=1.0 / 1024, bias=EPS), R=[ssq], W=[ssq])
                    P.op('vector', lambda e: e.reciprocal(out=ssq[:, 1:2], in_=ssq[:, 1:2]), R=[ssq], W=[ssq])
                    P.op('vector', lambda e, hs=hs: e.scalar_tensor_tensor(out=hs, in0=hs, scalar=ssq[:, 1:2], in1=gf[:], op0=ALU.mult, op1=ALU.mult), R=[htile, ssq, gf], W=[htile])
                P.dma('gpsimd', out[b0 * 128:(b0 + nb) * 128, :].rearrange("(j t) d -> t j d", t=128), htile[:, 0:nb, :], R=[htile])
            ffn_phase(ph, "d", ffn_tiles(False, NBO), h3, xn3T, [(wg1[e_], wu1[e_], wd1[e_], e_) for e_ in range(NE)], fin1)
            P.barrier(); P.emit()
        if upto <= 7:
            return nc
    return nc


def make_consts(NBO):
    NKB = 2 * NBO
    k = np.arange(128)[:, None]; q = np.arange(128)[None, :]
    c = {}
    c["c_ident"] = np.eye(128, dtype=np.float32)
    c["c_mown"] = np.where(k > q, NEG, 0.0).astype(np.float32)
    c["c_mprev"] = np.where(k <= q, NEG, 0.0).astype(np.float32)
    c["c_U"] = (k <= q).astype(np.float32)
    b = np.arange(NKB)
    c["c_SU"] = (b[:, None] < b[None, :]).astype(np.float32)
    c["c_eye"] = np.eye(NKB, dtype=np.float32)
    invm = (10000.0 ** (-np.arange(0, 32, 2, dtype=np.float32) / 32)).astype(np.float32)
    invs = (10000.0 ** (-np.arange(0, 64, 2, dtype=np.float32) / 64)).astype(np.float32)
    c["c_invm"] = np.broadcast_to(invm, (128, 16)).copy()
    c["c_invs"] = np.broadcast_to(invs, (128, 32)).copy()
    sh = np.zeros((4, 128, 128), np.float32)
    sh[0] = (k == q - 1); sh[1] = (k == q - 2); sh[2] = (k == q - 1 + 128); sh[3] = (k == q - 2 + 128)
    c["c_sh"] = sh
    return c


def fm(v, n):
    return np.ascontiguousarray(np.asarray(v, np.float32).reshape(n, 128).T)


def make_in_maps(inp, B, S):
    NBO = S // 2 // 128
    H = S // 2
    consts = make_consts(NBO)
    A = lambda k: np.ascontiguousarray(np.asarray(inp[k], np.float32))
    shared = dict(consts)
    shared.update(
        g0mix=fm(inp['l0_norm_mix'], 8), g0ffn=fm(inp['l0_norm_ffn'], 8), g1mix=fm(inp['l1_norm_mix'], 8), g1ffn=fm(inp['l1_norm_ffn'], 8),
        qng=fm(inp['l0_q_norm'], 3), kvng=fm(inp['l0_kv_norm'], 2),
        w_in0=A('l0_w_in'), bfor=A('l0_b_forget'), wqup=A('l0_w_q_up'), wkvup=A('l0_w_kv_up'),
        wout0=A('l0_w_out'), wg0=A('l0_w_gate'), wu0=A('l0_w_up'), wd0=A('l0_w_down'),
        w_in1=A('l1_w_in'), sinks=A('l1_sinks'), convw=A('l1_conv_w'), wout1=A('l1_w_out'),
        wrT=np.ascontiguousarray(np.asarray(inp['l1_w_router'], np.float32).T), g1ffn_row=A('l1_norm_ffn'), gfin_row=A('final_norm'),
        wg1=A('l1_w_gate'), wu1=A('l1_w_up'), wd1=A('l1_w_down'))
    x = np.asarray(inp['x'], np.float32); pos = np.asarray(inp['positions'], np.int32)
    maps = []
    for core in range(2 * B):
        b, half = core // 2, core % 2
        xk = np.zeros((S, 1024), np.float32); pk = np.zeros((S,), np.int32); valid = np.zeros((128, 2 * NBO), np.float32)
        if half == 0:
            xk[H:] = x[b, :H]; pk[H:] = pos[b, :H]
        else:
            xk[:] = x[b]; pk[:] = pos[b]; valid[:, :NBO] = 1.0
        valid[:, NBO:] = 1.0
        m = dict(shared)
        m.update(xk=xk, valid=valid, pos=np.ascontiguousarray(pk.reshape(2 * NBO, 128).T))
        maps.append(m)
    return maps


_NC_CACHE = {}


def kernel(**inputs):
    x = np.asarray(inputs['x'])
    B, S, D = x.shape
    NBO = S // 2 // 128
    if NBO not in _NC_CACHE:
        _NC_CACHE[NBO] = build(NBO)
    nc = _NC_CACHE[NBO]
    maps = make_in_maps(inputs, B, S)
    res = run_bass_kernel_spmd(nc, maps, core_ids=list(range(2 * B)))
    outp = np.zeros((B, S, D), np.float32)
    H = S // 2
    for core in range(2 * B):
        b, half = core // 2, core % 2
        outp[b, half * H:(half + 1) * H] = res.results[core]["out"]
    return outp
```
